# Optimizing a Trainium2 kernel written in Bass

```python
import jax, jax.numpy as jnp
from jax import lax
import numpy as np

D_MODEL = 2048
BATCH = 2
SEQ = 16384
DEPTH = 2

HEAD_DIM = 64
MOBA_HEADS = 8
NSA_HEADS = 8
NSA_KV_HEADS = 2
FOX_HEADS = 12
MEM_HEADS = 4
N_MEM = 256
MIX_WIDTH = (MOBA_HEADS + NSA_HEADS + FOX_HEADS + MEM_HEADS) * HEAD_DIM

Q_BLOCK = 128
MOBA_BLOCK = 256
MOBA_TOPK = 3
NSA_CMP_LEN = 32
NSA_CMP_STRIDE = 16
NSA_CMP_HIDDEN = 128
NSA_SEL_BLOCK = 64
NSA_TOPK = 16
NSA_WINDOW = 512
NSA_FORCE_SCORE = 1.0e4
D_FF = 5632
CONV_WIDTH = 3
LN_EPS = 1e-5
DEEPNORM_ALPHA = (2 * DEPTH) ** 0.25
DEEPNORM_BETA = (8 * DEPTH) ** -0.25

N_MOBA_QKV = 3 * MOBA_HEADS * HEAD_DIM
N_NSA_Q = NSA_HEADS * HEAD_DIM
N_NSA_KV = 6 * NSA_KV_HEADS * HEAD_DIM
N_NSA_GATE = 3 * NSA_HEADS
N_FOX_QKV = 3 * FOX_HEADS * HEAD_DIM
N_FOX_F = FOX_HEADS
N_MEM_Q = MEM_HEADS * HEAD_DIM
IN_SIZES = (N_MOBA_QKV, N_NSA_Q, N_NSA_KV, N_NSA_GATE, N_FOX_QKV, N_FOX_F, N_MEM_Q)
N_IN = N_MOBA_QKV + N_NSA_Q + N_NSA_KV + N_NSA_GATE + N_FOX_QKV + N_FOX_F + N_MEM_Q

kernel_name = "hybrid_moba_nsa_fox_deepnorm"

F32 = jnp.float32


def layer_norm(x, g, b):
    xf = x.astype(F32)
    mu = jnp.mean(xf, axis=-1, keepdims=True)
    var = jnp.mean(jnp.square(xf - mu), axis=-1, keepdims=True)
    y = (xf - mu) * lax.rsqrt(var + LN_EPS)
    return (y * g.astype(F32) + b.astype(F32)).astype(x.dtype)


def alibi_slopes(n):
    return jnp.exp2(-8.0 * jnp.arange(1, n + 1, dtype=F32) / n)


def masked_softmax(s, mask):
    s = jnp.where(mask, s, -jnp.inf)
    m = jnp.max(s, axis=-1, keepdims=True)
    m = jnp.where(jnp.isfinite(m), m, 0.0)
    e = jnp.where(mask, jnp.exp(s - m), 0.0)
    return e / jnp.maximum(jnp.sum(e, axis=-1, keepdims=True), 1e-30)


def moba_attention(q, k, v, slopes):
    B, H, S, Dh = q.shape
    scale = Dh ** -0.5
    s_pad = -(-S // MOBA_BLOCK) * MOBA_BLOCK
    nb = s_pad // MOBA_BLOCK
    n_sel = min(MOBA_TOPK, nb)
    pad = ((0, 0), (0, 0), (0, s_pad - S), (0, 0))
    k_p = jnp.pad(k, pad)
    v_p = jnp.pad(v, pad)
    k_blk = k_p.reshape(B, H, nb, MOBA_BLOCK, Dh)
    v_blk = v_p.reshape(B, H, nb, MOBA_BLOCK, Dh)
    k_mean = jnp.mean(k_blk.astype(F32), axis=3)
    bi = jnp.arange(B)[:, None, None, None]
    hi = jnp.arange(H)[None, :, None, None]
    blk_ids = jnp.arange(nb)
    off = jnp.arange(MOBA_BLOCK)

    def chunk(ci):
        c0 = ci * Q_BLOCK
        t = c0 + jnp.arange(Q_BLOCK)
        own = c0 // MOBA_BLOCK
        qc = lax.dynamic_slice_in_dim(q, c0, Q_BLOCK, axis=2)
        gate = jnp.einsum('bhqd,bhnd->bhqn', qc, k_mean, preferred_element_type=F32)
        gate = jnp.where(blk_ids < own, gate, -jnp.inf)
        _, sel = lax.top_k(gate, n_sel)
        sel_valid = sel < own
        k_sel = k_blk[bi, hi, sel]
        v_sel = v_blk[bi, hi, sel]
        pos_sel = sel[..., None] * MOBA_BLOCK + off
        s_sel = jnp.einsum('bhqd,bhqnld->bhqnl', qc, k_sel, preferred_element_type=F32) * scale
        s_sel = s_sel - slopes[:, None, None, None] * (t[:, None, None] - pos_sel).astype(F32)
        m_sel = jnp.broadcast_to(sel_valid[..., None], s_sel.shape)
        k_own = lax.dynamic_slice_in_dim(k_p, own * MOBA_BLOCK, MOBA_BLOCK, axis=2)
        v_own = lax.dynamic_slice_in_dim(v_p, own * MOBA_BLOCK, MOBA_BLOCK, axis=2)
        pos_own = own * MOBA_BLOCK + off
        d_own = t[:, None] - pos_own[None, :]
        s_own = jnp.einsum('bhqd,bhld->bhql', qc, k_own, preferred_element_type=F32) * scale
        s_own = s_own - slopes[:, None, None] * d_own.astype(F32)
        m_own = jnp.broadcast_to(d_own >= 0, s_own.shape)
        s_all = jnp.concatenate([s_own, s_sel.reshape(B, H, Q_BLOCK, -1)], axis=-1)
        m_all = jnp.concatenate([m_own, m_sel.reshape(B, H, Q_BLOCK, -1)], axis=-1)
        p = masked_softmax(s_all, m_all)
        p_own = p[..., :MOBA_BLOCK]
        p_sel = p[..., MOBA_BLOCK:].reshape(B, H, Q_BLOCK, n_sel, MOBA_BLOCK)
        o = (jnp.einsum('bhql,bhld->bhqd', p_own, v_own)
             + jnp.einsum('bhqnl,bhqnld->bhqd', p_sel, v_sel))
        return o.astype(q.dtype)

    out = lax.map(chunk, jnp.arange(S // Q_BLOCK))
    return out.transpose(1, 2, 0, 3, 4).reshape(B, H, S, Dh)


def nsa_compress(x, pe, w1, w2):
    B, S, G, Dh = x.shape
    xs = x.reshape(B, S // NSA_CMP_STRIDE, NSA_CMP_STRIDE, G, Dh)
    blk = jnp.concatenate([xs[:, :-1], xs[:, 1:]], axis=2)
    h = jax.nn.gelu(jnp.einsum('bnlgd,lde->bnge', blk + pe[None, None, :, None, :], w1))
    return jnp.einsum('bnge,ed->bgnd', h, w2)


def nsa_attention(q, k_cmp, v_cmp, k_slc, v_slc, k_win, v_win, gates, slopes):
    B, Hq, S, Dh = q.shape
    G = k_slc.shape[1]
    Hg = Hq // G
    nc = k_cmp.shape[2]
    nsb = S // NSA_SEL_BLOCK
    n_sel = min(NSA_TOPK, nsb)
    scale = Dh ** -0.5
    slopes_g = slopes.reshape(G, Hg)
    cmp_end = jnp.arange(nc) * NSA_CMP_STRIDE + NSA_CMP_LEN - 1
    ratio = NSA_SEL_BLOCK // NSA_CMP_STRIDE
    front = NSA_CMP_LEN // NSA_CMP_STRIDE - 1
    n_int = ratio + front
    back = ratio * (nsb - 1) + n_int - front - nc
    blk = jnp.arange(nsb)
    off = jnp.arange(NSA_SEL_BLOCK)
    k_sb = k_slc.reshape(B, G, nsb, NSA_SEL_BLOCK, Dh)
    v_sb = v_slc.reshape(B, G, nsb, NSA_SEL_BLOCK, Dh)
    wpad = ((0, 0), (0, 0), (NSA_WINDOW, 0), (0, 0))
    kw = jnp.pad(k_win, wpad)
    vw = jnp.pad(v_win, wpad)
    bi = jnp.arange(B)[:, None, None, None]
    gi = jnp.arange(G)[None, :, None, None]
    q_all = q.reshape(B, G, Hg, S, Dh)
    g_all = gates.reshape(B, G, Hg, S, 3)

    def chunk(ci):
        c0 = ci * Q_BLOCK
        t = c0 + jnp.arange(Q_BLOCK)
        qg = lax.dynamic_slice_in_dim(q_all, c0, Q_BLOCK, axis=3)
        gc = lax.dynamic_slice_in_dim(g_all, c0, Q_BLOCK, axis=3)
        d_c = t[:, None] - cmp_end[None, :]
        s_c = jnp.einsum('bghqd,bgnd->bghqn', qg, k_cmp, preferred_element_type=F32) * scale
        s_c = s_c - slopes_g[:, :, None, None] * d_c.astype(F32)
        p_c = masked_softmax(s_c, d_c >= 0)
        o_c = jnp.einsum('bghqn,bgnd->bghqd', p_c, v_cmp)
        imp_c = jnp.pad(jnp.sum(p_c, axis=2), ((0, 0), (0, 0), (0, 0), (front, back)))
        imp = imp_c[..., 0:ratio * (nsb - 1) + 1:ratio]
        for o in range(1, n_int):
            imp = imp + imp_c[..., o:o + ratio * (nsb - 1) + 1:ratio]
        jt = (t // NSA_SEL_BLOCK)[:, None]
        forced = (blk == 0) | (blk == jt) | (blk == jt - 1)
        imp = jnp.where(forced, NSA_FORCE_SCORE, imp)
        imp = jnp.where(blk * NSA_SEL_BLOCK <= t[:, None], imp, -jnp.inf)
        _, sel = lax.top_k(imp, n_sel)
        k_g = k_sb[bi, gi, sel]
        v_g = v_sb[bi, gi, sel]
        d_s = t[:, None, None] - (sel[..., None] * NSA_SEL_BLOCK + off)
        s_s = jnp.einsum('bghqd,bgqnld->bghqnl', qg, k_g, preferred_element_type=F32) * scale
        s_s = s_s - slopes_g[:, :, None, None, None] * d_s[:, :, None].astype(F32)
        shp = s_s.shape
        m_s = jnp.broadcast_to((d_s >= 0)[:, :, None], shp)
        p_s = masked_softmax(s_s.reshape(shp[:4] + (-1,)), m_s.reshape(shp[:4] + (-1,))).reshape(shp)
        o_s = jnp.einsum('bghqnl,bgqnld->bghqd', p_s, v_g)
        kwc = lax.dynamic_slice_in_dim(kw, c0, Q_BLOCK + NSA_WINDOW, axis=2)
        vwc = lax.dynamic_slice_in_dim(vw, c0, Q_BLOCK + NSA_WINDOW, axis=2)
        pos_w = c0 - NSA_WINDOW + jnp.arange(Q_BLOCK + NSA_WINDOW)
        d_w = t[:, None] - pos_w[None, :]
        m_w = (d_w >= 0) & (d_w < NSA_WINDOW) & (pos_w[None, :] >= 0)
        s_w = jnp.einsum('bghqd,bgkd->bghqk', qg, kwc, preferred_element_type=F32) * scale
        s_w = s_w - slopes_g[:, :, None, None] * d_w.astype(F32)
        p_w = masked_softmax(s_w, m_w)
        o_w = jnp.einsum('bghqk,bgkd->bghqd', p_w, vwc)
        o = gc[..., 0:1] * o_c + gc[..., 1:2] * o_s + gc[..., 2:3] * o_w
        return o.reshape(B, Hq, Q_BLOCK, Dh).astype(q.dtype)

    out = lax.map(chunk, jnp.arange(S // Q_BLOCK))
    return out.transpose(1, 2, 0, 3, 4).reshape(B, Hq, S, Dh)


def forgetting_attention(q, k, v, log_f):
    B, H, S, Dh = q.shape
    scale = Dh ** -0.5
    c = lax.cumsum(log_f, axis=2)
    s_pos = jnp.arange(S)

    def chunk(ci):
        c0 = ci * Q_BLOCK
        t = c0 + jnp.arange(Q_BLOCK)
        qc = lax.dynamic_slice_in_dim(q, c0, Q_BLOCK, axis=2)
        cq = lax.dynamic_slice_in_dim(c, c0, Q_BLOCK, axis=2)
        s = jnp.einsum('bhqd,bhkd->bhqk', qc, k, preferred_element_type=F32) * scale
        s = s + (cq[..., :, None] - c[:, :, None, :])
        p = masked_softmax(s, s_pos[None, :] <= t[:, None])
        return jnp.einsum('bhqk,bhkd->bhqd', p, v).astype(q.dtype)

    out = lax.map(chunk, jnp.arange(S // Q_BLOCK))
    return out.transpose(1, 2, 0, 3, 4).reshape(B, H, S, Dh)


def memory_attention(q, mem_k, mem_v):
    s = jnp.einsum('bhqd,bhmd->bhqm', q, mem_k, preferred_element_type=F32) * (q.shape[-1] ** -0.5)
    p = jax.nn.softmax(s, axis=-1)
    return jnp.einsum('bhqm,bhmd->bhqd', p, mem_v).astype(q.dtype)


def hybrid_mixer(x, mem, w_in, b_forget, w_mem_kv, cmp_pe, cmp_w1, cmp_w2, w_out):
    B, S, _ = x.shape
    Dh = HEAD_DIM
    proj = x @ w_in
    split_at = []
    acc = 0
    for n in IN_SIZES[:-1]:
        acc += n
        split_at.append(acc)
    moba_qkv, nsa_q, nsa_kv, nsa_g, fox_qkv, fox_f, mem_q = jnp.split(proj, split_at, axis=-1)

    mq, mk, mv = moba_qkv.reshape(B, S, 3, MOBA_HEADS, Dh).transpose(2, 0, 3, 1, 4)
    o_moba = moba_attention(mq, mk, mv, alibi_slopes(MOBA_HEADS))

    nq = nsa_q.reshape(B, S, NSA_HEADS, Dh).transpose(0, 2, 1, 3)
    kv = nsa_kv.reshape(B, S, 6, NSA_KV_HEADS, Dh)
    k_cmp = nsa_compress(kv[:, :, 0], cmp_pe[0], cmp_w1[0], cmp_w2[0])
    v_cmp = nsa_compress(kv[:, :, 1], cmp_pe[1], cmp_w1[1], cmp_w2[1])
    k_slc, v_slc, k_win, v_win = [kv[:, :, i].transpose(0, 2, 1, 3) for i in range(2, 6)]
    gates = jax.nn.sigmoid(nsa_g.reshape(B, S, NSA_HEADS, 3)).transpose(0, 2, 1, 3)
    o_nsa = nsa_attention(nq, k_cmp, v_cmp, k_slc, v_slc, k_win, v_win, gates,
                          alibi_slopes(NSA_HEADS))

    fq, fk, fv = fox_qkv.reshape(B, S, 3, FOX_HEADS, Dh).transpose(2, 0, 3, 1, 4)
    log_f = jax.nn.log_sigmoid((fox_f + b_forget).astype(F32)).transpose(0, 2, 1)
    o_fox = forgetting_attention(fq, fk, fv, log_f)

    eq = mem_q.reshape(B, S, MEM_HEADS, Dh).transpose(0, 2, 1, 3)
    ek, ev = (mem @ w_mem_kv).reshape(B, mem.shape[1], 2, MEM_HEADS, Dh).transpose(2, 0, 3, 1, 4)
    o_mem = memory_attention(eq, ek, ev)

    o = jnp.concatenate([o_moba, o_nsa, o_fox, o_mem], axis=1)
    o = o.transpose(0, 2, 1, 3).reshape(B, S, MIX_WIDTH)
    return o @ w_out


def conv_ffn(x, w_up, conv_w, conv_b, w_down):
    S = x.shape[1]
    u, g = jnp.split(x @ w_up, 2, axis=-1)
    gp = jnp.pad(g, ((0, 0), (CONV_WIDTH - 1, 0), (0, 0)))
    gc = conv_b
    for tap in range(CONV_WIDTH):
        gc = gc + conv_w[tap] * gp[:, tap:tap + S]
    return (jax.nn.gelu(gc) * u) @ w_down


def setup_inputs(seed: int = 0) -> dict:
    key = jax.random.key(seed)
    ks = jax.random.split(key, 20)

    def nrm(k, shape, scale):
        return jax.random.normal(k, shape, F32) * scale

    return {
        "x": nrm(ks[0], (BATCH, SEQ, D_MODEL), 1.0),
        "mem": nrm(ks[1], (BATCH, N_MEM, D_MODEL), 1.0),
        "emb_ln_g": 1.0 + nrm(ks[2], (D_MODEL,), 0.02),
        "emb_ln_b": nrm(ks[3], (D_MODEL,), 0.02),
        "w_in": nrm(ks[4], (DEPTH, D_MODEL, N_IN), D_MODEL ** -0.5),
        "b_forget": 3.0 + nrm(ks[5], (DEPTH, FOX_HEADS), 1.5),
        "w_mem_kv": nrm(ks[6], (DEPTH, D_MODEL, 2 * MEM_HEADS * HEAD_DIM), D_MODEL ** -0.5),
        "nsa_cmp_pe": nrm(ks[7], (DEPTH, 2, NSA_CMP_LEN, HEAD_DIM), 0.02),
        "nsa_cmp_w1": nrm(ks[8], (DEPTH, 2, NSA_CMP_LEN, HEAD_DIM, NSA_CMP_HIDDEN),
                          (NSA_CMP_LEN * HEAD_DIM) ** -0.5),
        "nsa_cmp_w2": nrm(ks[9], (DEPTH, 2, NSA_CMP_HIDDEN, HEAD_DIM), NSA_CMP_HIDDEN ** -0.5),
        "w_out": nrm(ks[10], (DEPTH, MIX_WIDTH, D_MODEL), MIX_WIDTH ** -0.5 * DEEPNORM_BETA),
        "ln1_g": 1.0 + nrm(ks[11], (DEPTH, D_MODEL), 0.02),
        "ln1_b": nrm(ks[12], (DEPTH, D_MODEL), 0.02),
        "ffn_w_up": nrm(ks[13], (DEPTH, D_MODEL, 2 * D_FF), D_MODEL ** -0.5),
        "ffn_conv_w": nrm(ks[14], (DEPTH, CONV_WIDTH, D_FF), CONV_WIDTH ** -0.5),
        "ffn_conv_b": nrm(ks[15], (DEPTH, D_FF), 0.02),
        "ffn_w_down": nrm(ks[16], (DEPTH, D_FF, D_MODEL), D_FF ** -0.5 * DEEPNORM_BETA),
        "ln2_g": 1.0 + nrm(ks[17], (DEPTH, D_MODEL), 0.02),
        "ln2_b": nrm(ks[18], (DEPTH, D_MODEL), 0.02),
    }


def reference(x, mem, emb_ln_g, emb_ln_b, w_in, b_forget, w_mem_kv, nsa_cmp_pe, nsa_cmp_w1,
              nsa_cmp_w2, w_out, ln1_g, ln1_b, ffn_w_up, ffn_conv_w, ffn_conv_b, ffn_w_down,
              ln2_g, ln2_b):
    h = layer_norm(x, emb_ln_g, emb_ln_b)
    for l in range(DEPTH):
        mix = hybrid_mixer(h, mem, w_in[l], b_forget[l], w_mem_kv[l], nsa_cmp_pe[l],
                           nsa_cmp_w1[l], nsa_cmp_w2[l], w_out[l])
        h = layer_norm(DEEPNORM_ALPHA * h + mix, ln1_g[l], ln1_b[l])
        ffn = conv_ffn(h, ffn_w_up[l], ffn_conv_w[l], ffn_conv_b[l], ffn_w_down[l])
        h = layer_norm(DEEPNORM_ALPHA * h + ffn, ln2_g[l], ln2_b[l])
    return h
```

```python
import os
import numpy as np
import ml_dtypes
from contextlib import ExitStack
import concourse.bass as bass
import concourse.mybir as mybir
from concourse.bass_utils import run_bass_kernel_spmd

F32 = mybir.dt.float32
BF16 = mybir.dt.bfloat16
AF = mybir.ActivationFunctionType
ALU = mybir.AluOpType
AX = mybir.AxisListType
NEG = -30000.0
BF = ml_dtypes.bfloat16


class _Stop(Exception):
    pass


def _ck(tag):
    if os.environ.get("K_STOP") == tag:
        raise _Stop()


class Cfg:
    def __init__(s, D=2048, S=16384, DEPTH=2, HM=8, HN=8, HF=12, HE=4, DFF=5632, NMEM=256):
        s.D, s.S, s.DEPTH, s.HM, s.HN, s.HF, s.HE, s.DFF, s.NMEM = D, S, DEPTH, HM, HN, HF, HE, DFF, NMEM
        s.G = 2
        s.HG = HN // 2
        s.KC = D // 128
        s.NR = 4
        s.CH = 512
        s.NSLOT = S // (s.NR * s.CH)
        s.NT = s.NSLOT * s.CH
        s.NTL = s.NT // 128
        s.FT = DFF // 128
        s.MIX = (HM + HN + HF + HE) * 64
        s.MKC = s.MIX // 128
        s.NCMP = S // 16
        s.NSB = S // 64
        s.NB = S // 256
        s.alpha = (2 * DEPTH) ** 0.25
        o = 0
        s.c_mq = o; o += HM * 64
        s.c_mk = o; o += HM * 64
        s.c_mv = o; o += HM * 64
        s.c_nq = o; o += HN * 64
        s.c_kc = o; o += 128
        s.c_vc = o; o += 128
        s.c_ks = o; o += 128
        s.c_vs = o; o += 128
        s.c_kw = o; o += 128
        s.c_vw = o; o += 128
        s.c_ng = o; o += 3 * HN
        s.c_fq = o; o += HF * 64
        s.c_fk = o; o += HF * 64
        s.c_fv = o; o += HF * 64
        s.c_ff = o; o += HF
        s.c_eq = o; o += HE * 64
        s.NIN = o
        o = 0
        s.rk_m = o; o += HM * 68
        s.rk_s = o; o += 2 * 68
        s.rk_w = o; o += 2 * 68
        s.rk_xk = o; o += 2 * 64
        s.rk_xv = o; o += 2 * 64
        s.rk_f = o; o += HF * 64
        s.RK = o
        s.NVH = HM + 4 + HF


class Res:
    __slots__ = ("w", "r", "name")

    def __init__(s, name=""):
        s.w = None
        s.r = {}
        s.name = name


class Sem:
    def __init__(s, h, kind, stream):
        s.h, s.kind, s.stream, s.count = h, kind, stream, 0


class Stream:
    def __init__(s, name):
        s.name = name
        s.q = []
        s.sems = {}
        s.waited = {}


class Prog:
    NDMA = 40

    def __init__(s, nc, es):
        s.nc = nc
        s.st = {n: Stream(n) for n in ("pe", "act", "dve", "pool", "sp")}
        for sn, kinds in (("pe", "c"), ("act", "c"), ("dve", "c"), ("pool", ("c", "d", "cc")), ("sp", "d")):
            for k in kinds:
                n = 1 if k == "c" else ((6 if sn == "pool" else 24) if k == "d" else 4)
                s.st[sn].sems[k] = [Sem(es.enter_context(nc.semaphore(f"s_{sn}_{k}{i}")), k, sn) for i in range(n)]
                s.st[sn].rr = getattr(s.st[sn], "rr", {})
                s.st[sn].rr[k] = 0
        s.ninst = 0

    def op(s, stream, kind, fn, rd=(), wr=()):
        st = s.st[stream]
        needs = {}

        def need(ev, raw):
            sem, val = ev
            if sem.stream == stream and sem.kind == "c":
                if stream == "pe":
                    return
            if needs.get(sem, 0) < val:
                needs[sem] = val

        for R in rd:
            if R.w is not None:
                need(R.w, True)
        for R in wr:
            if R.w is not None:
                need(R.w, False)
            for ev in R.r.items():
                need(ev, False)
        pool = st.sems[kind]
        sem = pool[st.rr[kind] % len(pool)]
        st.rr[kind] += 1
        if kind != "c" and sem.count > 0:
            needs[sem] = max(needs.get(sem, 0), sem.count)
        for sm, val in needs.items():
            if st.waited.get(sm, 0) < val:
                st.waited[sm] = val
                st.q.append(("w", sm.h, val))
        inc = 16 if kind == "d" else 1
        sem.count += inc
        st.q.append(("i", fn, sem.h, inc))
        ev = (sem, sem.count)
        for R in rd:
            if R.r.get(sem, 0) < sem.count:
                R.r[sem] = sem.count
        for R in wr:
            R.w = ev
            R.r = {}
        s.ninst += 1

    def barrier(s):
        allsems = [sm for st in s.st.values() for pl in st.sems.values() for sm in pl]
        for st in s.st.values():
            for sm in allsems:
                if sm.count > 0 and st.waited.get(sm, 0) < sm.count:
                    st.waited[sm] = sm.count
                    st.q.append(("w", sm.h, sm.count))

    def replay(s):
        nc = s.nc
        with nc.Block() as block:
            def run(q):
                def body(e):
                    for it in q:
                        if it[0] == "w":
                            e.wait_ge(it[1], it[2])
                        else:
                            ins = it[1](e)
                            if it[0] == "i" and it[3] == 16 or True:
                                ins.then_inc(it[2], it[3])
                return body
            block.tensor(run(s.st["pe"].q))
            block.scalar(run(s.st["act"].q))
            block.vector(run(s.st["dve"].q))
            block.gpsimd(run(s.st["pool"].q))
            block.sync(run(s.st["sp"].q))


class Arena:
    def __init__(s, t, n32):
        s.t, s.n32, s.off = t, n32, 0

    def reset(s):
        s.off = 0

    def get(s, shape, dt):
        part = shape[0]
        n = 1
        for d in shape[1:]:
            n *= d
        n32 = n if dt == F32 else (n + 1) // 2
        assert s.off + n32 <= s.n32, ("arena overflow", s.off, n32, s.n32)
        v = s.t[0:part, s.off:s.off + n32]
        s.off += n32
        if dt != F32:
            v = v.bitcast(dt)[:, 0:n]
        if len(shape) > 2:
            names = "abcdefg"[:len(shape) - 1]
            pat = "p (" + " ".join(names) + ") -> p " + " ".join(names)
            v = v.rearrange(pat, **{names[i]: shape[i + 1] for i in range(1, len(names))})
        return v


def dap(t, off, dims):
    return bass.AP(t.tensor, t.offset + off, [list(d) for d in dims])


def build(cfg):
    c = cfg
    nc = bass.Bass("TRN2", target_bir_lowering=False)
    es = ExitStack()
    P = Prog(nc, es)
    D, KC, NT, NSLOT, S = c.D, c.KC, c.NT, c.NSLOT, c.S
    HM, HN, HF, HE, G, HG = c.HM, c.HN, c.HF, c.HE, c.G, c.HG

    def din(name, shape, dt=F32):
        return nc.dram_tensor(name, list(shape), dt, kind="ExternalInput").ap()

    def dint(name, shape, dt):
        return nc.dram_tensor(name, list(shape), dt, kind="Internal").ap()

    xT = din("xT", [D, NT])
    memT = din("memT", [D, c.NMEM])
    wsh = {
        "in": din("w_in", [c.DEPTH, D // 4, c.NIN]),
        "out": din("w_out", [c.DEPTH, c.MIX // 4, D]),
        "up": din("w_up", [c.DEPTH, D // 4, 2 * c.DFF]),
        "down": din("w_down", [c.DEPTH, c.DFF // 4, D]),
        "mkv": din("w_mkv", [c.DEPTH, D // 4, 2 * HE * 64]),
    }
    wshape = {"in": (D, c.NIN), "out": (c.MIX, D), "up": (D, 2 * c.DFF), "down": (c.DFF, D), "mkv": (D, 2 * HE * 64)}
    lnp = din("lnp", [128, (2 + 4 * c.DEPTH) * KC])
    convp = din("convp", [128, c.DEPTH * 4 * c.FT])
    bfg = din("bfg", [HF, c.DEPTH])
    peT = din("peT", [64, c.DEPTH * 2 * 32])
    cw1 = din("cw1", [64, c.DEPTH * 2 * 32 * 128])
    cw2 = din("cw2", [128, c.DEPTH * 2 * 64])
    qpos = din("qpos", [HM + HN, 4, NT], BF16)
    kpos = din("kpos", [4, NT], BF16)
    kcpos = din("kcpos", [4, c.NCMP], BF16)
    fq3 = din("fq3", [3, NT], BF16)
    fk3 = din("fk3", [3, NT], BF16)
    cmt = din("cmt", [128, 16 * 512], BF16)
    wmt = din("wmt", [128, 20 * 512], BF16)
    cpm = din("cpm", [128, 2 * 512], BF16)
    cpmt = din("cpmt", [128, 4 * 256], BF16)
    post = din("post", [128, 2 * NSLOT * 4])
    iot = din("iot", [128, 64 + 64 + 256])
    identb = din("identb", [128, 128], BF16)
    ohsel = din("ohsel", [128, 8])
    out = nc.dram_tensor("out", [D, NT], F32, kind="ExternalOutput").ap()

    wl, wg, wseg = {}, {}, {}
    for k, (rows, cols) in wshape.items():
        if cols <= 4096:
            segs = [0, cols]
        elif k == "in":
            segs = [0, c.c_fq, cols]
        else:
            segs = list(range(0, cols, 4096)) + [cols]
        wseg[k] = segs
        for l in range(c.DEPTH):
            for si_ in range(len(segs) - 1):
                wl[k, l, si_] = dint(f"wl_{k}{l}_{si_}", [rows // 4, segs[si_ + 1] - segs[si_]], BF16)
                wg[k, l, si_] = dint(f"wg_{k}{l}_{si_}", [rows, segs[si_ + 1] - segs[si_]], BF16)

    def wview(k, l, c0, ncols):
        segs = wseg[k]
        for si_ in range(len(segs) - 1):
            if segs[si_] <= c0 and c0 + ncols <= segs[si_ + 1]:
                return wg[k, l, si_].rearrange("(k p) n -> p k n", p=128)[:, :, c0 - segs[si_]:c0 - segs[si_] + ncols]
        raise AssertionError(("weight block straddles segments", k, c0, ncols))
    hres = dint("hres", [D, NT], F32)
    hbf = dint("hbf", [D, NT], BF16)
    qM = dint("qM", [HM, 68, NT], BF16)
    qN = dint("qN", [HN, 68, NT], BF16)
    qF = dint("qF", [HF, 70, NT], BF16)
    qE = dint("qE", [HE, 64, NT], BF16)
    kl = dint("kl", [c.RK, NT], BF16)
    kg = dint("kg", [4 * c.RK, NT], BF16)
    vl = dint("vl", [c.NVH * 128, c.NTL * 65], BF16)
    vg = dint("vg", [4 * c.NVH * 128, c.NTL * 65], BF16)
    kgroups = [(c.rk_m + h * 68, 68) for h in range(HM)] + [(c.rk_s, 68), (c.rk_s + 68, 68), (c.rk_w, 68), (c.rk_w + 68, 68),
               (c.rk_xk, 128), (c.rk_xv, 128)] + [(c.rk_f + h0 * 64, min(2, HF - h0) * 64) for h0 in range(0, HF, 2)]

    def kg_ap(r, row0, nrows):
        for (g0, gn) in kgroups:
            if g0 <= row0 and row0 + nrows <= g0 + gn:
                base = 4 * g0 + r * gn + (row0 - g0)
                return kg[base:base + nrows, :]
        raise AssertionError(("k rows straddle groups", row0, nrows))

    def vg_ap(r, vh):
        base = (4 * vh + r) * 128
        return vg[base:base + 128, :].rearrange("p (t e) -> p t e", e=65)
    lfl = dint("lfl", [HF, NT], F32)
    lfg = dint("lfg", [4 * HF, NT], F32)
    KMW = max(16, NSLOT * 2)
    kml = dint("kml", [HM * 64, KMW], BF16)
    kmg = dint("kmg", [4 * HM * 64, KMW], BF16)
    cF = dint("cF", [HF, 3, S], BF16)
    kC = dint("kC", [G, 68, c.NCMP], BF16)
    vC = dint("vC", [G, 128, (c.NCMP // 128) * 65], BF16)
    kE = dint("kE", [HE, 64, c.NMEM], BF16)
    vE = dint("vE", [HE, 128, (c.NMEM // 128) * 65], BF16)
    oT = dint("oT", [c.MIX, NT], BF16)
    gT = dint("gT", [3 * HN, NT], F32)
    hal_l = dint("hal_l", [D, NSLOT * 2], BF16)
    hal_g = dint("hal_g", [4 * D, NSLOT * 2], BF16)
    RG = [[0, 1, 2, 3], [4, 5, 6, 7]]

    A32 = 42500
    arena_t = es.enter_context(nc.sbuf_tensor("arena", [128, A32], F32))
    cst_t = es.enter_context(nc.sbuf_tensor("cst", [128, 2200], F32))
    AR = Arena(arena_t, A32)
    CS = Arena(cst_t, 2200)
    ps = [es.enter_context(nc.psum_tensor(f"ps{i}", [128, 512], F32)) for i in range(8)]
    psR = [Res(f"ps{i}") for i in range(8)]

    def mm(o, lhsT, rhs, start, stop, rd, wr):
        P.op("pe", "c", lambda e: e.matmul(o, lhsT=lhsT, rhs=rhs, start=start, stop=stop), rd, wr)

    def tr(o, i, rd, wr):
        P.op("pe", "c", lambda e: e.transpose(o, i, ident[0:i.shape[0], 0:i.shape[0]]), rd + [R_cst], wr)

    def act(o, i, func, rd, wr, bias=None, scale=None, accum=None):
        kw = {}
        if bias is not None:
            kw["bias"] = bias
        if scale is not None:
            kw["scale"] = scale
        if accum is not None:
            kw["accum_out"] = accum
        P.op("act", "c", lambda e: e.activation(out=o, in_=i, func=func, **kw), rd, wr)

    def V(name, rd, wr, *a, **k):
        P.op("dve", "c", lambda e: getattr(e, name)(*a, **k), rd, wr)

    def PL(name, rd, wr, *a, **k):
        P.op("pool", "c", lambda e: getattr(e, name)(*a, **k), rd, wr)

    def dma(q, o, i, rd, wr):
        if q == "pool" and o.dtype == i.dtype:
            q = "sp"
        P.op(q, "d", lambda e: e.dma_start(out=o, in_=i), rd, wr)

    def gather(src, dst, rd, wr):
        P.op("pool", "cc", lambda e: e.collective_compute("AllGather", ALU.bypass, replica_groups=RG, ins=[src], outs=[dst]), rd, wr)

    R_cst = Res("cst")
    ident = CS.get([128, 128], BF16)
    lnp_s = CS.get([128, (2 + 4 * c.DEPTH) * KC], F32)
    convp_s = CS.get([128, c.DEPTH * 4 * c.FT], F32)
    post_s = CS.get([128, 2 * NSLOT * 4], F32)
    iot_s = CS.get([128, 384], F32)
    ohs = CS.get([128, 8], F32)
    bfg_s = CS.get([HF, c.DEPTH], F32)
    ones32 = CS.get([128, 128], F32)
    onesb = CS.get([128, 64], BF16)
    eps_s = CS.get([128, 1], F32)
    for o, i in ((ident, identb), (lnp_s, lnp), (convp_s, convp), (post_s, post), (iot_s, iot), (ohs, ohsel), (bfg_s, bfg)):
        dma("sp", o, i, [], [R_cst])
    PL("memset", [], [R_cst], ones32, 1.0)
    PL("memset", [], [R_cst], onesb, 1.0)
    PL("memset", [], [R_cst], eps_s, 1e-5)
    P.barrier()

    try:
        R_w = {}
        AR.reset()
        stg = [AR.get([128, 4096], BF16) for _ in range(2)]
        stgR = [Res("wst0"), Res("wst1")]
        it = 0
        for l in range(c.DEPTH):
            for k, (rows, cols) in wshape.items():
                R_w[k, l] = Res(f"w_{k}{l}")
                src = wsh[k][l]
                rl = rows // 4
                CR = min(128, rl)
                assert rl % CR == 0
                segs = wseg[k]
                for a in range(rl // CR):
                    for si_ in range(len(segs) - 1):
                        Rl = Res()
                        for c0 in range(segs[si_], segs[si_ + 1], 4096):
                            w = min(4096, segs[si_ + 1] - c0)
                            b = it % 2
                            it += 1
                            for c1 in range(0, w, 2048):
                                w1 = min(2048, w - c1)
                                dma("pool", stg[b][0:CR, c1:c1 + w1], src[a * CR:(a + 1) * CR, c0 + c1:c0 + c1 + w1], [], [stgR[b]])
                            dma("sp", wl[k, l, si_][a * CR:(a + 1) * CR, c0 - segs[si_]:c0 - segs[si_] + w], stg[b][0:CR, 0:w], [stgR[b]], [Rl])
                        gather(wl[k, l, si_][a * CR:(a + 1) * CR, :], wg[k, l, si_][4 * a * CR:4 * (a + 1) * CR, :], [Rl], [R_w[k, l]])
        P.barrier()
        _ck("w")

        R_hres, R_hbf, R_out = Res("hres"), Res("hbf"), Res("out")

        def ln_chunk(y, yR, goff, boff, store32, dstb, dstRb, pA, pB, L):
            sq, sqR, st32, st32R, rs, rsR = L
            for kc in range(KC):
                mm(ps[pA][:, :], ones32, y[:, kc, :], kc == 0, kc == KC - 1, [yR, R_cst], [psR[pA]])
            for kc in range(KC):
                b = kc % 2
                act(sq[b], y[:, kc, :], AF.Square, [yR], [sqR[b]])
                mm(ps[pB][:, :], ones32, sq[b], kc == 0, kc == KC - 1, [sqR[b], R_cst], [psR[pB]])
            mean, var = rs[:, 0, :], rs[:, 1, :]
            act(mean, ps[pA][:, :], AF.Copy, [psR[pA]], [rsR], scale=1.0 / D)
            V("tensor_tensor", [rsR], [rsR], var, mean, mean, ALU.mult)
            V("scalar_tensor_tensor", [rsR, psR[pB]], [rsR], var, ps[pB][:, :], 1.0 / D, var, ALU.mult, ALU.subtract)
            act(var, var, AF.Sqrt, [rsR, R_cst], [rsR], bias=eps_s[:, 0:1])
            V("reciprocal", [rsR], [rsR], var, var)
            for kc in range(KC):
                b = kc % 2
                V("tensor_tensor", [yR, rsR], [sqR[b]], sq[b], y[:, kc, :], mean, ALU.subtract)
                V("tensor_tensor", [sqR[b], rsR], [sqR[b]], sq[b], sq[b], var, ALU.mult)
                act(st32[b], sq[b], AF.Identity, [sqR[b], R_cst], [st32R[b]],
                    scale=lnp_s[:, goff + kc:goff + kc + 1], bias=lnp_s[:, boff + kc:boff + kc + 1])
                V("tensor_copy", [st32R[b]], [dstRb], dstb[:, kc, :], st32[b])
                store32(kc, st32[b], st32R[b])

        def ln_bufs():
            return ([AR.get([128, 512], F32) for _ in range(2)], [Res(), Res()],
                    [AR.get([128, 512], F32) for _ in range(2)], [Res(), Res()],
                    AR.get([128, 2, 512], F32), Res())

        hview = hres.rearrange("(k p) t -> p k t", p=128)
        hbview = hbf.rearrange("(k p) t -> p k t", p=128)
        outview = out.rearrange("(k p) t -> p k t", p=128)

        AR.reset()
        xs = AR.get([128, KC, 512], F32)
        xsR = Res()
        ob = AR.get([128, KC, 512], BF16)
        obR = Res()
        L0 = ln_bufs()
        xview = xT.rearrange("(k p) t -> p k t", p=128)
        for j in range(NSLOT):
            sl = slice(j * 512, (j + 1) * 512)
            dma("sp", xs, xview[:, :, sl], [], [xsR])
            ln_chunk(xs, xsR, 0, KC, lambda kc, ap, r_, sl=sl: dma("sp", hview[:, kc, sl], ap, [r_], [R_hres]), ob, obR, 0, 1, L0)
            dma("sp", hbview[:, :, sl], ob, [obR], [R_hbf])
        P.barrier()
        _ck("0")

        R_q, R_kl, R_kg, R_vl, R_vg = Res("q"), Res("kl"), Res("kg"), Res("vl"), Res("vg")
        R_lfl, R_lfg, R_kml, R_kmg = Res(), Res(), Res(), Res()
        R_cF, R_kC, R_vC, R_kE, R_vE, R_oT, R_gT = Res(), Res(), Res(), Res(), Res(), Res(), Res()
        R_hal_l, R_hal_g = Res(), Res()

        for l in range(c.DEPTH):
            AR.reset()
            hTc = [AR.get([128, KC, 512], BF16) for _ in range(2)]
            hTcR = [Res(), Res()]
            WB = [AR.get([128, KC, 512], BF16) for _ in range(2)]
            WBR = [Res(), Res()]
            evs = [AR.get([128, 512], BF16) for _ in range(4)]
            evR = [Res() for _ in range(4)]
            ev32 = [AR.get([128, 512], F32) for _ in range(2)]
            ev32R = [Res() for _ in range(2)]
            vst = [AR.get([128, 8, 4, 65], BF16) for _ in range(2)]
            vstR = [Res(), Res()]
            kmst = AR.get([128, (HM + 1) // 2, KMW], F32)
            kmstb = AR.get([128, (HM + 1) // 2, KMW], BF16)
            PL("memset", [], [Res()], kmstb, 0.0)
            kmR = Res()
            for b in range(2):
                PL("memset", [], [vstR[b]], vst[b], 1.0)
            pst = [AR.get([4, NT], BF16) for _ in range(2)]
            pstR = [Res(), Res()]
            pq = [0]

            def put_rows(src, dsts, dres):
                b = pq[0] % 2
                pq[0] += 1
                n = src.shape[0]
                dma("sp", pst[b][0:n, :], src, [], [pstR[b]])
                for d in dsts:
                    dma("pool", d, pst[b][0:n, :], [pstR[b]], [dres])

            put_rows(kpos, [kl[c.rk_m + h * 68 + 64:c.rk_m + h * 68 + 68, :] for h in range(HM)]
                     + [kl[c.rk_s + g * 68 + 64:c.rk_s + g * 68 + 68, :] for g in range(2)]
                     + [kl[c.rk_w + g * 68 + 64:c.rk_w + g * 68 + 68, :] for g in range(2)], R_kl)
            for h in range(HM):
                put_rows(qpos[h], [qM[h, 64:68, :]], R_q)
            for h in range(HN):
                put_rows(qpos[HM + h], [qN[h, 64:68, :]], R_q)
            put_rows(fq3, [qF[h, 67:70, :] for h in range(HF)], R_q)
            _ck("Aa")

            blocks = []

            def fm_heads(col, nh, kind, h0):
                tiles = []
                for t0 in range(0, nh, 2):
                    n = min(2, nh - t0)
                    tiles.append((col + t0 * 64, n * 64, kind, h0 + t0))
                return tiles

            fm_all = []
            fm_all += fm_heads(c.c_mq, HM, "qM", 0) + fm_heads(c.c_mk, HM, "kM", 0)
            fm_all += fm_heads(c.c_nq, HN, "qN", 0)
            fm_all += [(c.c_kc, 128, "xk", 0), (c.c_vc, 128, "xv", 0), (c.c_ks, 128, "kS", 0), (c.c_kw, 128, "kW", 0)]
            fm_all += [(c.c_ng, 3 * HN, "gate", 0)]
            fm_all += fm_heads(c.c_fq, HF, "qF", 0) + fm_heads(c.c_fk, HF, "kF", 0)
            fm_all += [(c.c_ff, HF, "ff", 0)]
            fm_all += fm_heads(c.c_eq, HE, "qE", 0)
            tm_all = []
            for h0 in range(0, HM, 8):
                tm_all.append((c.c_mv + h0 * 64, min(8, HM - h0) * 64, h0, min(8, HM - h0)))
            tm_all.append((c.c_vs, 128, HM, 2))
            tm_all.append((c.c_vw, 128, HM + 2, 2))
            for h0 in range(0, HF, 8):
                tm_all.append((c.c_fv + h0 * 64, min(8, HF - h0) * 64, HM + 4 + h0, min(8, HF - h0)))
            items = sorted([(t[0], t[1], "fm", t) for t in fm_all] + [(t[0], t[1], "tm", t) for t in tm_all])
            cur = None
            for it_ in items:
                if cur is None or it_[0] + it_[1] - cur[0] > 512 or (c.NIN > 4096 and cur[0] < c.c_fq <= it_[0]):
                    cur = [it_[0], 0, []]
                    blocks.append(cur)
                cur[1] = it_[0] + it_[1] - cur[0]
                cur[2].append(it_)

            def kdest(kind, hh):
                if kind == "kM":
                    return kl[c.rk_m + hh * 68:c.rk_m + hh * 68 + 64, :]
                if kind == "kF":
                    return kl[c.rk_f + hh * 64:c.rk_f + hh * 64 + 64, :]
                if kind == "kS":
                    return kl[c.rk_s + hh * 68:c.rk_s + hh * 68 + 64, :]
                if kind == "kW":
                    return kl[c.rk_w + hh * 68:c.rk_w + hh * 68 + 64, :]
                if kind == "xk":
                    return kl[c.rk_xk + hh * 64:c.rk_xk + hh * 64 + 64, :]
                if kind == "xv":
                    return kl[c.rk_xv + hh * 64:c.rk_xv + hh * 64 + 64, :]
                if kind == "qM":
                    return qM[hh, 0:64, :]
                if kind == "qN":
                    return qN[hh, 0:64, :]
                if kind == "qF":
                    return qF[hh, 0:64, :]
                if kind == "qE":
                    return qE[hh, 0:64, :]
                raise KeyError(kind)

            pi = 0
            ei = 0
            vi = 0
            vlv = vl.rearrange("(h p) (t e) -> p h t e", p=128, e=65)
            wbi = 0
            for j in range(NSLOT):
                jb = j % 2
                tsl = slice(j * 512, (j + 1) * 512)
                dma("sp", hTc[jb], hbview[:, :, tsl], [R_hbf], [hTcR[jb]])
                for bi, (bc0, bw, bitems) in enumerate(blocks):
                    wb = wbi % 2
                    wbi += 1
                    dma("sp", WB[wb][:, :, 0:bw], wview("in", l, bc0, bw), [R_w["in", l]], [WBR[wb]])
                    for (_, _, typ, t) in bitems:
                        SK = os.environ.get("K_SKIP", "")
                        if typ == "tm" and "t" in SK:
                            continue
                        if typ == "fm" and t[2] in ("gate", "ff") and "g" in SK:
                            continue
                        if typ == "fm" and t[2] not in ("gate", "ff") and "f" in SK:
                            continue
                        if typ == "fm":
                            col, nrow, kind, h0 = t
                            pb = pi % 6
                            pi += 1
                            for kc in range(KC):
                                mm(ps[pb][0:nrow, :], WB[wb][:, kc, col - bc0:col - bc0 + nrow], hTc[jb][:, kc, :],
                                   kc == 0, kc == KC - 1, [WBR[wb], hTcR[jb]], [psR[pb]])
                            if kind == "gate":
                                e = ei % 2
                                ei += 1
                                act(ev32[e][0:nrow, :], ps[pb][0:nrow, :], AF.Sigmoid, [psR[pb]], [ev32R[e]])
                                dma("pool", gT[:, tsl], ev32[e][0:nrow, :], [ev32R[e]], [R_gT])
                            elif kind == "ff":
                                e = ei % 2
                                ei += 1
                                act(ev32[e][0:nrow, :], ps[pb][0:nrow, :], AF.Sigmoid, [psR[pb], R_cst], [ev32R[e]],
                                    bias=bfg_s[:, l:l + 1])
                                act(ev32[e][0:nrow, :], ev32[e][0:nrow, :], AF.Ln, [ev32R[e]], [ev32R[e]])
                                dma("pool", lfl[:, tsl], ev32[e][0:nrow, :], [ev32R[e]], [R_lfl])
                            else:
                                e = ei % 4
                                ei += 1
                                isk = kind[0] == "k"
                                if isk:
                                    act(evs[e][0:nrow, :], ps[pb][0:nrow, :], AF.Copy, [psR[pb]], [evR[e]], scale=0.125)
                                else:
                                    V("tensor_copy", [psR[pb]], [evR[e]], evs[e][0:nrow, :], ps[pb][0:nrow, :])
                                if kind == "kM" and "k" not in os.environ.get("K_SKIP", ""):
                                    for bb in range(2):
                                        act(ev32[0][0:nrow, 0:256], ps[pb][0:nrow, bb * 256:(bb + 1) * 256], AF.Copy, [psR[pb]], [ev32R[0], kmR],
                                            scale=0.125 / 256.0, accum=kmst[0:nrow, h0 // 2, 2 * j + bb:2 * j + bb + 1])
                                    if j == NSLOT - 1:
                                        V("tensor_copy", [kmR], [kmR], kmstb[0:nrow, h0 // 2, 0:NSLOT * 2], kmst[0:nrow, h0 // 2, 0:NSLOT * 2])
                                        for hh in range(nrow // 64):
                                            dma("pool", kml[(h0 + hh) * 64:(h0 + hh) * 64 + 64, :], kmstb[hh * 64:hh * 64 + 64, h0 // 2, :],
                                                [kmR], [R_kml])
                                if kind in ("kS", "kW", "xk", "xv"):
                                    hl = [0, 1]
                                else:
                                    hl = list(range(nrow // 64))
                                Rd = R_kl if (isk or kind[0] == "x") else R_q
                                for hh in hl:
                                    dma("pool", kdest(kind, h0 + hh)[:, tsl], evs[e][hh * 64:hh * 64 + 64, :], [evR[e]], [Rd])
                        else:
                            col, ncol, vh0, nh = t
                            vb = vi % 2
                            vi += 1
                            for tt in range(4):
                                pb = pi % 6
                                pi += 1
                                for kc in range(KC):
                                    mm(ps[pb][:, 0:ncol], hTc[jb][:, kc, tt * 128:tt * 128 + 128],
                                       WB[wb][:, kc, col - bc0:col - bc0 + ncol], kc == 0, kc == KC - 1, [WBR[wb], hTcR[jb]], [psR[pb]])
                                V("tensor_copy", [psR[pb]], [vstR[vb]], vst[vb][:, 0:nh, tt, 0:64],
                                  ps[pb][:, 0:ncol].rearrange("p (h e) -> p h e", e=64))
                            for hh in range(nh):
                                dma("pool", vlv[:, vh0 + hh, 4 * j:4 * j + 4, :], vst[vb][:, hh, :, :], [vstR[vb]], [R_vl])
            _ck("Ab")
            memb = AR.get([128, KC, c.NMEM], BF16)
            membR = Res()
            dma("pool", memb, memT.rearrange("(k p) t -> p k t", p=128), [], [membR])
            Wm = AR.get([128, KC, 2 * HE * 64], BF16)
            WmR = Res()
            dma("sp", Wm, wview("mkv", l, 0, 2 * HE * 64), [R_w["mkv", l]], [WmR])
            for t0 in range(0, HE, 2):
                n = min(2, HE - t0) * 64
                pb = pi % 6
                pi += 1
                for kc in range(KC):
                    mm(ps[pb][0:n, 0:c.NMEM], Wm[:, kc, t0 * 64:t0 * 64 + n], memb[:, kc, :], kc == 0, kc == KC - 1, [WmR, membR], [psR[pb]])
                e = ei % 4
                ei += 1
                act(evs[e][0:n, 0:c.NMEM], ps[pb][0:n, 0:c.NMEM], AF.Copy, [psR[pb]], [evR[e]], scale=0.125)
                for hh in range(n // 64):
                    dma("pool", kE[t0 + hh], evs[e][hh * 64:hh * 64 + 64, 0:c.NMEM], [evR[e]], [R_kE])
            vEv = vE.rearrange("h p (t e) -> p h t e", e=65)
            for tt in range(c.NMEM // 128):
                pb = pi % 6
                pi += 1
                for kc in range(KC):
                    mm(ps[pb][:, 0:HE * 64], memb[:, kc, tt * 128:(tt + 1) * 128], Wm[:, kc, HE * 64:2 * HE * 64],
                       kc == 0, kc == KC - 1, [WmR, membR], [psR[pb]])
                vb = vi % 2
                vi += 1
                V("tensor_copy", [psR[pb]], [vstR[vb]], vst[vb][:, 0:HE, 0, 0:64], ps[pb][:, 0:HE * 64].rearrange("p (h e) -> p h e", e=64))
                for hh in range(HE):
                    dma("pool", vEv[:, hh, tt, :], vst[vb][:, hh, 0, :], [vstR[vb]], [R_vE])
            _ck("Ac")
            for (g0, gn) in kgroups:
                gather(kl[g0:g0 + gn, :], kg[4 * g0:4 * g0 + 4 * gn, :], [R_kl], [R_kg])
            for vh in range(c.NVH):
                gather(vl[vh * 128:(vh + 1) * 128, :], vg[4 * vh * 128:4 * (vh + 1) * 128, :], [R_vl], [R_vg])
            gather(lfl, lfg, [R_lfl], [R_lfg])
            gather(kml, kmg, [R_kml], [R_kmg])
            P.barrier()
            _ck("A%d" % l)

            AR.reset()
            Cc = AR.get([HF, S], F32)
            CcR = Res()
            for r in range(4):
                dma("sp", Cc.rearrange("p (j r w) -> p j r w", r=4, w=512)[:, :, r, :],
                    lfg[r * HF:(r + 1) * HF, :].rearrange("p (j w) -> p j w", w=512), [R_lfg], [CcR])
            PIECE = 2048
            for p0 in range(0, S, PIECE):
                init = 0.0 if p0 == 0 else Cc[:, p0 - 1:p0]
                V("tensor_tensor_scan", [CcR], [CcR], Cc[:, p0:p0 + PIECE], Cc[:, p0:p0 + PIECE], Cc[:, p0:p0 + PIECE], init, ALU.add, ALU.bypass)
            Co = AR.get([HF, NT], F32)
            CoR = Res()
            Ccv = Cc.rearrange("p (j r w) -> p j r w", r=4, w=512)
            Cov = Co.rearrange("p (j w) -> p j w", w=512)
            V("tensor_scalar", [CcR, R_cst], [CoR], Cov, Ccv[:, :, 0, :], ohs[0:HF, 0:1], None, ALU.mult)
            for r in range(1, 4):
                V("scalar_tensor_tensor", [CcR, CoR, R_cst], [CoR], Cov, Ccv[:, :, r, :], ohs[0:HF, r:r + 1], Cov, ALU.mult, ALU.add)

            def split3(src, srcR, n, dsts, dR):
                cb = AR.get([HF, n], BF16)
                cbR = Res()
                for i in range(3):
                    V("tensor_copy", [srcR], [cbR], cb, src)
                    dma("pool", dsts[i], cb, [cbR], [dR])
                    if i < 2:
                        V("tensor_tensor", [srcR, cbR], [srcR], src, src, cb, ALU.subtract)

            split3(Cc, CcR, S, [cF[:, i, :] for i in range(3)], R_cF)
            split3(Co, CoR, NT, [qF[:, 64 + i, :] for i in range(3)], R_q)
            P.barrier()
            _ck("B1_%d" % l)

            AR.reset()
            NCH = max(1, c.NCMP // 512)
            ncw = min(512, c.NCMP)
            w1s = AR.get([64, 2, 32, 128], BF16)
            w1R = Res()
            dma("pool", w1s, cw1[:, l * 2 * 32 * 128:(l + 1) * 2 * 32 * 128].rearrange("p (a b c) -> p a b c", b=32, c=128), [], [w1R])
            w2s = AR.get([128, 2, 64], BF16)
            dma("pool", w2s, cw2[:, l * 128:(l + 1) * 128].rearrange("p (a b) -> p a b", b=64), [], [w1R])
            pes = AR.get([64, 2, 32], BF16)
            dma("pool", pes, peT[:, l * 64:(l + 1) * 64].rearrange("p (a b) -> p a b", b=32), [], [w1R])
            XT = [AR.get([64, S + 32], BF16) for _ in range(2)]
            XTR = [Res(), Res()]
            hg = [AR.get([128, 512], BF16) for _ in range(2)]
            hgR = [Res(), Res()]
            bcol = AR.get([128, 4], F32)
            bcR = Res()
            kcs = AR.get([64, 512], BF16)
            kcsR = Res()
            vcs = AR.get([128, 4, 65], BF16)
            vcsR = Res()
            PL("memset", [], [vcsR], vcs, 1.0)
            kcps = AR.get([4, c.NCMP], BF16)
            kcpsR = Res()
            kgv = kg.rearrange("(r k) t -> r k t", r=4)
            vCv = vC.rearrange("g p (t e) -> g p t e", e=65)
            xi = 0
            for g in range(G):
                dma("sp", kcps, kcpos, [], [kcpsR])
                dma("pool", kC[g, 64:68, :], kcps, [kcpsR], [R_kC])
                for kv in range(2):
                    xb = xi % 2
                    xi += 1
                    row0 = (c.rk_xk if kv == 0 else c.rk_xv) + g * 64
                    PL("memset", [], [XTR[xb]], XT[xb][:, S:S + 32], 0.0)
                    for r in range(4):
                        dma("sp", XT[xb][:, 0:S].rearrange("p (j r w) -> p j r w", r=4, w=512)[:, :, r, :],
                            kg_ap(r, row0, 64).rearrange("p (j w) -> p j w", w=512), [R_kg], [XTR[xb]])
                    for li in range(32):
                        mm(ps[6][:, 0:1], w1s[:, kv, li, :], pes[:, kv, li:li + 1], li == 0, li == 31, [w1R], [psR[6]])
                    V("tensor_copy", [psR[6]], [bcR], bcol[:, 2 * g + kv:2 * g + kv + 1], ps[6][:, 0:1])
                    for nchk in range(NCH):
                        pb = nchk % 2
                        for li in range(32):
                            rhs = dap(XT[xb], nchk * ncw * 16 + li, [[XT[xb].ap[0][0], 64], [16, ncw]])
                            mm(ps[pb][:, 0:ncw], w1s[:, kv, li, :], rhs, li == 0, li == 31, [w1R, XTR[xb]], [psR[pb]])
                        hb = nchk % 2
                        act(hg[hb][:, 0:ncw], ps[pb][:, 0:ncw], AF.Gelu_apprx_tanh, [psR[pb], bcR], [hgR[hb]],
                            bias=bcol[:, 2 * g + kv:2 * g + kv + 1])
                        if kv == 0:
                            mm(ps[2][0:64, 0:ncw], w2s[:, 0, :], hg[hb][:, 0:ncw], True, True, [w1R, hgR[hb]], [psR[2]])
                            act(kcs[:, 0:ncw], ps[2][0:64, 0:ncw], AF.Copy, [psR[2]], [kcsR], scale=0.125)
                            dma("pool", kC[g, 0:64, nchk * ncw:(nchk + 1) * ncw], kcs[:, 0:ncw], [kcsR], [R_kC])
                        else:
                            nt_ = ncw // 128
                            for tt in range(nt_):
                                mm(ps[3][:, tt * 64:(tt + 1) * 64], hg[hb][:, tt * 128:(tt + 1) * 128], w2s[:, 1, :], True, True,
                                   [w1R, hgR[hb]], [psR[3]])
                            V("tensor_copy", [psR[3]], [vcsR], vcs[:, 0:nt_, 0:64], ps[3][:, 0:nt_ * 64].rearrange("p (t e) -> p t e", e=64))
                            dma("pool", vCv[g, :, nchk * nt_:(nchk + 1) * nt_, :], vcs[:, 0:nt_, :], [vcsR], [R_vC])
            P.barrier()
            _ck("B%d" % l)

            AR.reset()
            cm_s = AR.get([128, 16, 512], BF16)
            wm_s = AR.get([128, 20, 512], BF16)
            cp_s = AR.get([128, 2, 512], BF16)
            cpt_s = AR.get([128, 4, 256], BF16)
            mR = Res("masks")
            dma("sp", cm_s, cmt.rearrange("p (a b) -> p a b", b=512), [], [mR])
            dma("sp", wm_s, wmt.rearrange("p (a b) -> p a b", b=512), [], [mR])
            dma("sp", cp_s, cpm.rearrange("p (a b) -> p a b", b=512), [], [mR])
            dma("sp", cpt_s, cpmt.rearrange("p (a b) -> p a b", b=256), [], [mR])
            Kt = AR.get([70, 4, NT], BF16)
            KtR = Res("Kt")
            Vt = AR.get([128, 4, c.NTL, 65], BF16)
            VtR = Res("Vt")
            Kw = [AR.get([68, 5, 512], BF16) for _ in range(2)]
            KwR = [Res(), Res()]
            Vw = [AR.get([128, 5, 4, 65], BF16) for _ in range(2)]
            VwR = [Res(), Res()]
            Qc = [AR.get([70, 512], BF16) for _ in range(3)]
            QcR = [Res() for _ in range(3)]
            Pt = [AR.get([128, 512], BF16) for _ in range(3)]
            PtR = [Res() for _ in range(3)]
            Osb = AR.get([65, 512], F32)
            OsbR = Res()
            frow = AR.get([65, 512], F32)
            frowR = Res()
            grow = AR.get([65, 512], F32)
            growR = Res()
            onorm = AR.get([64, 512], BF16)
            onormR = Res()
            oacc = AR.get([64, 512], F32)
            oaccR = Res()
            otmp = AR.get([64, 512], F32)
            otmpR = Res()
            MBT = AR.get([128, 512], BF16)
            MBTR = Res()
            KMt = AR.get([64, c.NB], BF16)
            KMR = Res()
            selw = AR.get([128, 1100], F32)
            selR = Res()
            selb = AR.get([128, 1100], F32)
            selbR = Res()
            acc_imp = AR.get([128, 1040], F32)
            accR = Res()
            m8 = AR.get([128, 16], F32)
            m8R = Res()
            mbq = AR.get([128, 256], BF16)
            mbqR = Res()
            vb_t = AR.get([128, 64], F32)
            no_t = AR.get([128, 64], F32)
            vbR = Res()
            rinv = AR.get([128, 4], F32)
            rinvR = Res()
            PL("memset", [], [frowR], frow, 1.0)
            PS = ident.ap[0][0]

            kgv = kg.rearrange("(r k) t -> r k t", r=4)
            vgv = vg.rearrange("(r h p) (t e) -> r h p t e", r=4, p=128, e=65)
            cFv = cF.rearrange("h i (j r w) -> h i j r w", r=4, w=512)

            qi = [0]
            sq = [0]
            oq = [0]

            def load_kv(krow0, nrows, vh, fox_h=None):
                for r in range(4):
                    dma("sp", Kt[0:nrows, r, :], kg_ap(r, krow0, nrows), [R_kg], [KtR])
                    dma("sp", Vt[:, r, :, :], vg_ap(r, vh), [R_vg], [VtR])
                if fox_h is not None:
                    for r in range(4):
                        dma("sp", Kt[64:67, r, :], fk3, [], [KtR])
                        dma("sp", Kt[67:70, r, :].rearrange("p (j w) -> p j w", w=512), cFv[fox_h, :, :, r, :], [R_cF], [KtR])

            def load_q(src, nrows, tsl):
                b = qi[0] % 3
                qi[0] += 1
                dma("sp", Qc[b][0:nrows, :], src[0:nrows, tsl], [R_q], [QcR[b]])
                return Qc[b], QcR[b]

            def ktile_ap(nrows, g):
                ch, sub = g // 4, g % 4
                r, j = ch % 4, ch // 4
                return Kt[0:nrows, r, j * 512 + sub * 128:j * 512 + sub * 128 + 128], Vt[:, r, 4 * j + sub, :]

            def attend(qap, qR, tiles, out_cb):
                ob = 4 + (oq[0] % 2)
                oq[0] += 1
                n = len(tiles)
                for i, (kap, vap, kR, vR, extra) in enumerate(tiles):
                    sb = sq[0] % 4
                    sq[0] += 1
                    pt = sb % 3
                    mm(ps[sb][:, :], kap, qap, True, len(extra) == 0, [kR, qR], [psR[sb]])
                    for ei_, (r0, nr, xl, xr, xres) in enumerate(extra):
                        later = any(not (e2[0] + e2[1] <= r0 or e2[0] >= r0 + nr) for e2 in extra[ei_ + 1:])
                        mm(ps[sb][r0:r0 + nr, :], xl, xr, False, not later, xres, [psR[sb]])
                    act(Pt[pt], ps[sb][:, :], AF.Exp, [psR[sb]], [PtR[pt]])
                    mm(ps[ob][0:65, :], vap, Pt[pt], i == 0, i == n - 1, [vR, PtR[pt]], [psR[ob]])
                out_cb(ob)

            def finish(ob, gate_row=None, first=True, last=True, orow=None, tsl=None):
                act(Osb[0:64, :], ps[ob][0:64, :], AF.Copy, [psR[ob]], [OsbR])
                V("tensor_scalar", [psR[ob]], [frowR], frow[64:65, :], ps[ob][64:65, :], 1e-30, None, ALU.max)
                V("reciprocal", [frowR], [frowR], frow[64:65, :], frow[64:65, :])
                if gate_row is not None:
                    dma("sp", grow[64:65, :], gT[gate_row:gate_row + 1, tsl], [R_gT], [growR])
                    V("tensor_tensor", [frowR, growR], [frowR], frow[64:65, :], frow[64:65, :], grow[64:65, :], ALU.mult)
                mm(ps[6][0:64, :], ones32[64:65, 0:64], frow[64:65, :], True, True, [R_cst, frowR], [psR[6]])
                if gate_row is None:
                    V("tensor_tensor", [OsbR, psR[6]], [onormR], onorm, Osb[0:64, :], ps[6][0:64, :], ALU.mult)
                    dma("pool", oT[orow:orow + 64, tsl], onorm, [onormR], [R_oT])
                else:
                    if first:
                        V("tensor_tensor", [OsbR, psR[6]], [oaccR], oacc, Osb[0:64, :], ps[6][0:64, :], ALU.mult)
                    else:
                        V("tensor_tensor", [OsbR, psR[6]], [otmpR], otmp, Osb[0:64, :], ps[6][0:64, :], ALU.mult)
                        V("tensor_tensor", [oaccR, otmpR], [oaccR], oacc, oacc, otmp, ALU.add)
                    if last:
                        V("tensor_copy", [oaccR], [onormR], onorm, oacc)
                        dma("pool", oT[orow:orow + 64, tsl], onorm, [onormR], [R_oT])

            def causal_tiles(nrows, j, extra_fn=None):
                tiles = []
                for g in range(16 * (j + 1)):
                    kap, vap = ktile_ap(nrows, g)
                    extra = []
                    if extra_fn is not None:
                        extra += extra_fn(g)
                    if g >= 16 * j:
                        extra.append((0, 128, ident, cm_s[:, g - 16 * j, :], [R_cst, mR]))
                    tiles.append((kap, vap, KtR, VtR, extra))
                return tiles

            for h in range(HF):
                load_kv(c.rk_f + h * 64, 64, HM + 4 + h, fox_h=h)
                for j in range(NSLOT):
                    tsl = slice(j * 512, (j + 1) * 512)
                    q_, qR_ = load_q(qF[h], 70, tsl)
                    attend(q_[0:70, :], qR_, causal_tiles(70, j),
                           lambda ob, h=h, tsl=tsl: finish(ob, orow=(HM + HN + h) * 64, tsl=tsl))

            kmv = kmg.rearrange("(r h d) b -> r h d b", r=4, d=64)
            for h in range(HM):
                load_kv(c.rk_m + h * 68, 68, h)
                for r in range(4):
                    dma("sp", KMt.rearrange("p (j r b) -> p j r b", r=4, b=2)[:, :, r, :], kmv[r, h][:, 0:NSLOT * 2].rearrange("p (j b) -> p j b", b=2),
                        [R_kmg], [KMR])
                for j in range(NSLOT):
                    tsl = slice(j * 512, (j + 1) * 512)
                    q_, qR_ = load_q(qM[h], 68, tsl)
                    nbv = 8 * (j + 1)
                    for qt in range(4):
                        col = j * 4 + qt
                        mm(ps[7][:, 0:nbv], q_[0:64, qt * 128:qt * 128 + 128], KMt[:, 0:nbv], True, True, [qR_, KMR], [psR[7]])
                        V("tensor_scalar", [R_cst], [vbR], vb_t[:, 0:nbv], iot_s[:, 0:nbv], post_s[:, col:col + 1], -1e30, ALU.is_gt, ALU.mult)
                        V("tensor_scalar", [R_cst], [vbR], no_t[:, 0:nbv], iot_s[:, 64:64 + nbv], post_s[:, col:col + 1], None, ALU.not_equal)
                        V("memset", [], [selR], selw[:, 0:64], -3e38)
                        V("tensor_tensor", [psR[7], vbR], [selR], selw[:, 0:nbv], ps[7][:, 0:nbv], vb_t[:, 0:nbv], ALU.add)
                        V("max", [selR], [m8R], m8[:, 0:8], selw[:, 0:64])
                        V("tensor_scalar", [selR, m8R], [selbR], selb[:, 0:nbv], selw[:, 0:nbv], m8[:, 2:3], NEG, ALU.is_lt, ALU.mult)
                        V("tensor_tensor", [selbR, vbR], [mbqR], mbq[:, 0:nbv], selb[:, 0:nbv], no_t[:, 0:nbv], ALU.mult)
                        tr(ps[7].bitcast(BF16)[0:nbv, 512:640], mbq[:, 0:nbv], [mbqR], [psR[7]])
                        act(MBT[0:nbv, qt * 128:(qt + 1) * 128], ps[7].bitcast(BF16)[0:nbv, 512:640], AF.Copy, [psR[7]], [MBTR])

                    def mextra(g, nbv=nbv):
                        return [(0, 128, dap(ident, g // 2, [[PS, nbv], [0, 128]]), MBT[0:nbv, :], [R_cst, MBTR])]

                    attend(q_[0:68, :], qR_, causal_tiles(68, j, mextra),
                           lambda ob, h=h, tsl=tsl: finish(ob, orow=h * 64, tsl=tsl))

            kEs = AR.get([64, HE, c.NMEM], BF16)
            vEs = AR.get([128, HE, c.NMEM // 128, 65], BF16)
            kER = Res()
            dma("sp", kEs, kE.rearrange("h p t -> p h t"), [R_kE], [kER])
            dma("sp", vEs, vE.rearrange("h p (t e) -> p h t e", e=65), [R_vE], [kER])
            for h in range(HE):
                for j in range(NSLOT):
                    tsl = slice(j * 512, (j + 1) * 512)
                    q_, qR_ = load_q(qE[h], 64, tsl)
                    tiles = [(kEs[:, h, t * 128:(t + 1) * 128], vEs[:, h, t, :], kER, kER, []) for t in range(c.NMEM // 128)]
                    attend(q_[0:64, :], qR_, tiles,
                           lambda ob, h=h, tsl=tsl: finish(ob, orow=(HM + HN + HF + h) * 64, tsl=tsl))

            kCs = AR.get([68, G, c.NCMP], BF16)
            vCs = AR.get([128, G, c.NCMP // 128, 65], BF16)
            kCR = Res()
            dma("sp", kCs, kC.rearrange("g p t -> p g t"), [R_kC], [kCR])
            dma("sp", vCs, vC.rearrange("g p (t e) -> p g t e", e=65), [R_vC], [kCR])
            QN = [[AR.get([68, 512], BF16) for _ in range(HG)] for _ in range(2)]
            QNR = [[Res() for _ in range(HG)] for _ in range(2)]
            MB2 = [AR.get([128, 2, 512], BF16) for _ in range(2)]
            MB2R = [Res() for _ in range(2)]
            nbh = max(1, c.NSB // 128)
            nbp = min(128, c.NSB)
            si = 0
            for g in range(G):
                load_kv(c.rk_s + g * 68, 68, HM + g)
                for j in range(NSLOT):
                    sb_ = si % 2
                    si += 1
                    tsl = slice(j * 512, (j + 1) * 512)
                    for hh in range(HG):
                        dma("sp", QN[sb_][hh], qN[g * HG + hh][:, tsl], [R_q], [QNR[sb_][hh]])
                    for r in range(4):
                        dma("sp", Kw[sb_][:, 1 + r, :], kg_ap(r, c.rk_w + g * 68, 68)[:, tsl], [R_kg], [KwR[sb_]])
                        dma("sp", Vw[sb_][:, 1 + r, :, :], vg_ap(r, HM + 2 + g)[:, 4 * j:4 * j + 4, :], [R_vg], [VwR[sb_]])
                    if j > 0:
                        psl = slice((j - 1) * 512, j * 512)
                        dma("sp", Kw[sb_][:, 0, :], kg_ap(3, c.rk_w + g * 68, 68)[:, psl], [R_kg], [KwR[sb_]])
                        dma("sp", Vw[sb_][:, 0, :, :], vg_ap(3, HM + 2 + g)[:, 4 * (j - 1):4 * j, :], [R_vg], [VwR[sb_]])
                    ncv = 128 * (j + 1)
                    nbv = 32 * (j + 1)
                    for qt in range(4):
                        col = NSLOT * 4 + j * 4 + qt
                        qs = slice(qt * 128, qt * 128 + 128)
                        V("memset", [], [accR], acc_imp, 0.0)
                        for hh in range(HG):
                            for n0 in range(0, ncv, 512):
                                nw = min(512, ncv - n0)
                                pb = (n0 // 512) % 2
                                extra = []
                                for blk in range(n0 // 128, (n0 + nw) // 128):
                                    if blk >= j - 1:
                                        extra.append(blk)
                                mm(ps[pb][:, 0:nw], QN[sb_][hh][0:68, qs], kCs[:, g, n0:n0 + nw], True, len(extra) == 0,
                                   [QNR[sb_][hh], kCR], [psR[pb]])
                                for xi_, blk in enumerate(extra):
                                    mm(ps[pb][:, blk * 128 - n0:blk * 128 - n0 + 128], ident,
                                       cpt_s[:, qt, (blk - (j - 1)) * 128:(blk - (j - 1)) * 128 + 128],
                                       False, xi_ == len(extra) - 1, [R_cst, mR], [psR[pb]])
                                act(selw[:, n0:n0 + nw], ps[pb][:, 0:nw], AF.Exp, [psR[pb]], [selR], accum=m8[:, 8 + n0 // 512:9 + n0 // 512])
                            if ncv > 512:
                                V("tensor_tensor", [selR], [selR], m8[:, 8:9], m8[:, 8:9], m8[:, 9:10], ALU.add)
                            V("tensor_scalar", [selR], [rinvR], rinv[:, 0:1], m8[:, 8:9], 1e-30, None, ALU.max)
                            V("reciprocal", [rinvR], [rinvR], rinv[:, 0:1], rinv[:, 0:1])
                            V("scalar_tensor_tensor", [selR, rinvR, accR], [accR], acc_imp[:, 1:1 + ncv], selw[:, 0:ncv], rinv[:, 0:1],
                              acc_imp[:, 1:1 + ncv], ALU.mult, ALU.add)
                        pv = lambda o: dap(acc_imp, o, [[acc_imp.ap[0][0], 128], [4, nbv]])
                        V("tensor_tensor", [accR], [selbR], selb[:, 0:nbv], pv(0), pv(1), ALU.add)
                        for o in (2, 3, 4):
                            V("tensor_tensor", [accR, selbR], [selbR], selb[:, 0:nbv], selb[:, 0:nbv], pv(o), ALU.add)
                        io64 = iot_s[:, 128:128 + nbv]
                        t64 = post_s[:, col:col + 1]
                        A_ = selw[:, 0:nbv]
                        B_ = selw[:, 256:256 + nbv]
                        C_ = selw[:, 512:512 + nbv]
                        V("tensor_scalar", [R_cst], [selR], A_, io64, t64, -64.0, ALU.subtract, ALU.is_ge)
                        V("tensor_scalar", [R_cst], [selR], B_, io64, t64, None, ALU.is_le)
                        V("scalar_tensor_tensor", [selR], [selR], A_, A_, 1e4, B_, ALU.mult, ALU.mult)
                        V("tensor_tensor", [selR, selbR], [selbR], selb[:, 0:nbv], selb[:, 0:nbv], A_, ALU.max)
                        V("memset", [], [selbR], selb[:, 0:1], 1e4)
                        V("tensor_scalar", [selR], [selR], C_, B_, -1.0, 1e30, ALU.add, ALU.mult)
                        V("tensor_tensor", [selR, selbR], [selbR], selb[:, 0:nbv], selb[:, 0:nbv], B_, ALU.mult)
                        V("tensor_tensor", [selR, selbR], [selbR], selb[:, 0:nbv], selb[:, 0:nbv], C_, ALU.add)
                        if nbv < 256:
                            V("memset", [], [selbR], selb[:, nbv:256], -3e38)
                        V("max", [selbR], [m8R], m8[:, 0:8], selb[:, 0:256])
                        V("match_replace", [selbR, m8R], [selR], selw[:, 0:256], m8[:, 0:8], selb[:, 0:256], -3e38)
                        V("max", [selR], [m8R], m8[:, 0:8], selw[:, 0:256])
                        V("tensor_scalar", [selbR, m8R], [mbqR], mbq[:, 0:256], selb[:, 0:256], m8[:, 7:8], NEG, ALU.is_lt, ALU.mult)
                        for hf in range(min(nbh, (nbv + 127) // 128)):
                            tr(ps[7].bitcast(BF16)[0:nbp, 512:640], mbq[:, hf * 128:hf * 128 + nbp], [mbqR], [psR[7]])
                            act(MB2[sb_][0:nbp, hf, qt * 128:(qt + 1) * 128], ps[7].bitcast(BF16)[0:nbp, 512:640], AF.Copy, [psR[7]], [MB2R[sb_]])
                    for hh in range(HG):
                        h = g * HG + hh
                        orow = (HM + h) * 64
                        qap, qR_ = QN[sb_][hh][0:68, :], QNR[sb_][hh]
                        tiles = []
                        for t in range(j + 1):
                            extra = []
                            if t >= j - 1:
                                extra.append((0, 128, ident, cp_s[:, t - (j - 1), :], [R_cst, mR]))
                            tiles.append((kCs[:, g, t * 128:(t + 1) * 128], vCs[:, g, t, :], kCR, kCR, extra))
                        attend(qap, qR_, tiles,
                               lambda ob, h=h, tsl=tsl, orow=orow: finish(ob, gate_row=3 * h + 0, first=True, last=False, orow=orow, tsl=tsl))

                        def sextra(gk, sb_=sb_):
                            half, loc = (2 * gk) // 128, (2 * gk) % 128
                            return [(0, 64, dap(ident, loc, [[PS, nbp], [0, 64]]), MB2[sb_][0:nbp, half, :], [R_cst, MB2R[sb_]]),
                                    (64, 64, dap(ident, loc + 1, [[PS, nbp], [0, 64]]), MB2[sb_][0:nbp, half, :], [R_cst, MB2R[sb_]])]

                        attend(qap, qR_, causal_tiles(68, j, sextra),
                               lambda ob, h=h, tsl=tsl, orow=orow: finish(ob, gate_row=3 * h + 1, first=False, last=False, orow=orow, tsl=tsl))
                        tiles = []
                        for wi in range(20):
                            gk = 16 * j - 4 + wi
                            if gk < 0:
                                continue
                            tiles.append((Kw[sb_][:, wi // 4, (wi % 4) * 128:(wi % 4) * 128 + 128], Vw[sb_][:, wi // 4, wi % 4, :],
                                          KwR[sb_], VwR[sb_], [(0, 128, ident, wm_s[:, wi, :], [R_cst, mR])]))
                        attend(qap, qR_, tiles,
                               lambda ob, h=h, tsl=tsl, orow=orow: finish(ob, gate_row=3 * h + 2, first=False, last=True, orow=orow, tsl=tsl))
            P.barrier()
            _ck("C%d" % l)
            raise_if = None
            AR.reset()
            MKC, FT = c.MKC, c.FT
            WBk = [AR.get([128, 16, 512], BF16) for _ in range(3)]
            WBkR = [Res() for _ in range(3)]
            wq = [0]

            def wload(wk, nk, k0, c0, ncols, wres):
                b = wq[0] % 3
                wq[0] += 1
                dma("sp", WBk[b][:, 0:nk, 0:ncols], wview(wk, l, c0, ncols)[:, k0:k0 + nk, :], [wres], [WBkR[b]])
                return b

            oTs = AR.get([128, MKC, 512], BF16)
            oTsR = Res()
            hc = AR.get([128, KC, 512], F32)
            hcR = Res()
            h1b = AR.get([128, KC, 514], BF16)
            h1bR = Res()
            LD = ln_bufs()
            oTv = oT.rearrange("(k p) t -> p k t", p=128)
            Woutv, Wupv, Wdownv = "out", "up", "down"
            g1, b1 = (2 + 4 * l) * KC, (3 + 4 * l) * KC
            g2, b2 = (4 + 4 * l) * KC, (5 + 4 * l) * KC
            hallv = hal_l.rearrange("(k p) (j e) -> p k j e", p=128, e=2)
            h1store = dint(f"h1s{l}", [D, NT], F32)
            h1bstore = dint(f"h1b{l}", [D, NT], BF16)
            R_h1s, R_h1b = Res(), Res()
            h1sv = h1store.rearrange("(k p) t -> p k t", p=128)
            h1bv = h1bstore.rearrange("(k p) t -> p k t", p=128)
            for j in range(NSLOT):
                tsl = slice(j * 512, (j + 1) * 512)
                dma("sp", oTs, oTv[:, :, tsl], [R_oT], [oTsR])
                dma("sp", hc, hview[:, :, tsl], [R_hres], [hcR])
                for n0 in range(0, D, 512):
                    nw = min(512, D - n0)
                    nkp = [(k0, min(16, MKC - k0)) for k0 in range(0, MKC, 16)]
                    for nt_ in range(nw // 128):
                        pass
                    bufs = [(wload(Woutv, nk, k0, n0, nw, R_w["out", l]), k0, nk) for (k0, nk) in nkp]
                    for nt_ in range(nw // 128):
                        pb = nt_ % 4
                        first = True
                        for (b, k0, nk) in bufs:
                            for kk in range(nk):
                                mm(ps[pb][:, :], WBk[b][:, kk, nt_ * 128:(nt_ + 1) * 128], oTs[:, k0 + kk, :], first,
                                   (k0 + kk) == MKC - 1, [WBkR[b], oTsR], [psR[pb]])
                                first = False
                        kc = n0 // 128 + nt_
                        V("scalar_tensor_tensor", [hcR, psR[pb]], [hcR], hc[:, kc, :], hc[:, kc, :], c.alpha, ps[pb][:, :], ALU.mult, ALU.add)
                ln_chunk(hc, hcR, g1, b1, lambda kc, ap, r_, tsl=tsl: dma("sp", h1sv[:, kc, tsl], ap, [r_], [R_h1s]),
                         h1b[:, :, 2:514], h1bR, 6, 7, LD)
                dma("pool", h1bv[:, :, tsl], h1b[:, :, 2:514], [h1bR], [R_h1b])
                dma("pool", hallv[:, :, j, :], h1b[:, :, 512:514], [h1bR], [R_hal_l])
            gather(hal_l, hal_g, [R_hal_l], [R_hal_g])
            P.barrier()
            AR.reset()
            WBk = [AR.get([128, 16, 512], BF16) for _ in range(3)]
            WBkR = [Res() for _ in range(3)]
            hc = AR.get([128, KC, 512], F32)
            hcR = Res()
            h1b = AR.get([128, KC, 514], BF16)
            h1bR = Res()
            LD = ln_bufs()
            aT = AR.get([128, FT, 512], BF16)
            aTR = Res()
            ug = AR.get([128, 2, 514], F32)
            ugR = Res()
            gc = AR.get([128, 512], F32)
            gcR = Res()
            hg_all = AR.get([128, KC, 4, NSLOT, 2], BF16)
            hgaR = Res()
            halo = AR.get([128, KC, NSLOT, 2], BF16)
            haloR = Res()
            for r in range(4):
                dma("sp", hg_all[:, :, r, :, :], hal_g.rearrange("(r k p) (j e) -> r p k j e", r=4, p=128, e=2)[r], [R_hal_g], [hgaR])
            V("memset", [], [haloR], halo, 0.0)
            for r in range(3):
                V("scalar_tensor_tensor", [hgaR, haloR, R_cst], [haloR], halo, hg_all[:, :, r, :, :], ohs[:, 4 + r:5 + r], halo, ALU.mult, ALU.add)
            if NSLOT > 1:
                V("scalar_tensor_tensor", [hgaR, haloR, R_cst], [haloR], halo[:, :, 1:NSLOT, :], hg_all[:, :, 3, 0:NSLOT - 1, :], ohs[:, 7:8],
                  halo[:, :, 1:NSLOT, :], ALU.mult, ALU.add)
            cvo = l * 4 * FT
            for j in range(NSLOT):
                tsl = slice(j * 512, (j + 1) * 512)
                dma("sp", h1b[:, :, 2:514], h1bv[:, :, tsl], [R_h1b], [h1bR])
                V("tensor_copy", [haloR], [h1bR], h1b[:, :, 0:2], halo[:, :, j, :])
                dma("sp", hc, h1sv[:, :, tsl], [R_h1s], [hcR])
                for f0 in range(0, c.DFF, 512):
                    fw = min(512, c.DFF - f0)
                    bu = wload(Wupv, KC, 0, f0, fw, R_w["up", l])
                    bg = wload(Wupv, KC, 0, c.DFF + f0, fw, R_w["up", l])
                    for ft_ in range(fw // 128):
                        fi = f0 // 128 + ft_
                        pu, pg, ph = 0 + 2 * (fi % 2), 1 + 2 * (fi % 2), 4 + (fi % 2)
                        for kc in range(KC):
                            mm(ps[pu][:, :], WBk[bu][:, kc, ft_ * 128:(ft_ + 1) * 128], h1b[:, kc, 2:514], kc == 0, kc == KC - 1, [WBkR[bu], h1bR], [psR[pu]])
                        for kc in range(KC):
                            mm(ps[pg][:, :], WBk[bg][:, kc, ft_ * 128:(ft_ + 1) * 128], h1b[:, kc, 2:514], kc == 0, kc == KC - 1, [WBkR[bg], h1bR], [psR[pg]])
                        for kc in range(KC):
                            mm(ps[ph][:, 0:2], WBk[bg][:, kc, ft_ * 128:(ft_ + 1) * 128], h1b[:, kc, 0:2], kc == 0, kc == KC - 1, [WBkR[bg], h1bR], [psR[ph]])
                        act(ug[:, 1, 2:514], ps[pg][:, :], AF.Copy, [psR[pg]], [ugR])
                        act(ug[:, 1, 0:2], ps[ph][:, 0:2], AF.Copy, [psR[ph]], [ugR])
                        cw = lambda tap: convp_s[:, cvo + tap * FT + fi:cvo + tap * FT + fi + 1]
                        V("tensor_scalar", [ugR, R_cst], [gcR], gc, ug[:, 1, 0:512], cw(0), cw(3), ALU.mult, ALU.add)
                        V("scalar_tensor_tensor", [ugR, gcR, R_cst], [gcR], gc, ug[:, 1, 1:513], cw(1), gc, ALU.mult, ALU.add)
                        V("scalar_tensor_tensor", [ugR, gcR, R_cst], [gcR], gc, ug[:, 1, 2:514], cw(2), gc, ALU.mult, ALU.add)
                        act(gc, gc, AF.Gelu_apprx_tanh, [gcR], [gcR])
                        V("tensor_tensor", [gcR, psR[pu]], [aTR], aT[:, fi, :], gc, ps[pu][:, :], ALU.mult)
                for n0 in range(0, D, 512):
                    nw = min(512, D - n0)
                    nkp = [(k0, min(16, FT - k0)) for k0 in range(0, FT, 16)]
                    for pi_, (k0, nk) in enumerate(nkp):
                        b = wload(Wdownv, nk, k0, n0, nw, R_w["down", l])
                        for nt_ in range(nw // 128):
                            pb = nt_ % 4
                            for kk in range(nk):
                                mm(ps[pb][:, :], WBk[b][:, kk, nt_ * 128:(nt_ + 1) * 128], aT[:, k0 + kk, :], (k0 + kk) == 0,
                                   (k0 + kk) == FT - 1, [WBkR[b], aTR], [psR[pb]])
                    for nt_ in range(nw // 128):
                        pb = nt_ % 4
                        kc = n0 // 128 + nt_
                        V("scalar_tensor_tensor", [hcR, psR[pb]], [hcR], hc[:, kc, :], hc[:, kc, :], c.alpha, ps[pb][:, :], ALU.mult, ALU.add)
                if l == c.DEPTH - 1:
                    st_ = lambda kc, ap, r_, tsl=tsl: dma("sp", outview[:, kc, tsl], ap, [r_], [R_out])
                else:
                    st_ = lambda kc, ap, r_, tsl=tsl: dma("sp", hview[:, kc, tsl], ap, [r_], [R_hres])
                ln_chunk(hc, hcR, g2, b2, st_, h1b[:, :, 2:514], h1bR, 6, 7, LD)
                if l != c.DEPTH - 1:
                    dma("sp", hbview[:, :, tsl], h1b[:, :, 2:514], [h1bR], [R_hbf])
            P.barrier()
            _ck("D%d" % l)


    except _Stop:
        P.barrier()
    P.replay()
    es.close()
    return nc, P.ninst


def host_inputs(c, inp):
    f32 = np.float32
    in_maps = []
    S, NT, NSLOT, D, KC = c.S, c.NT, c.NSLOT, c.D, c.KC
    HM, HN, HF = c.HM, c.HN, c.HF

    def fm(v):
        v = np.asarray(v, f32)
        k = v.shape[-1] // 128
        return np.moveaxis(v.reshape(v.shape[:-1] + (k, 128)), -1, 0)

    lnp = [fm(inp["emb_ln_g"]), fm(inp["emb_ln_b"])]
    for l in range(c.DEPTH):
        lnp += [fm(inp["ln1_g"][l]), fm(inp["ln1_b"][l]), fm(inp["ln2_g"][l]), fm(inp["ln2_b"][l])]
    lnp = np.ascontiguousarray(np.concatenate(lnp, axis=1))
    cv = []
    for l in range(c.DEPTH):
        for tap in range(3):
            cv.append(fm(inp["ffn_conv_w"][l, tap]))
        cv.append(fm(inp["ffn_conv_b"][l]))
    convp = np.ascontiguousarray(np.concatenate(cv, axis=1))
    bfg = np.ascontiguousarray(np.asarray(inp["b_forget"], f32).T)
    peT = np.ascontiguousarray(np.transpose(np.asarray(inp["nsa_cmp_pe"], f32), (3, 0, 1, 2)).reshape(64, -1))
    cw1 = np.ascontiguousarray(np.transpose(np.asarray(inp["nsa_cmp_w1"], f32), (3, 0, 1, 2, 4)).reshape(64, -1))
    cw2 = np.ascontiguousarray(np.transpose(np.asarray(inp["nsa_cmp_w2"], f32), (2, 0, 1, 3)).reshape(128, -1))
    slopes_m = np.exp2(-8.0 * np.arange(1, HM + 1) / HM)
    slopes_n = np.exp2(-8.0 * np.arange(1, HN + 1) / HN)
    slopes = np.concatenate([slopes_m, slopes_n])
    ce = 16 * np.arange(c.NCMP) + 31
    kcpos = np.stack([np.ones(c.NCMP), np.ones(c.NCMP), (ce // 128) * 128, ce % 128]).astype(BF)
    iot = np.zeros((128, 384), f32)
    iot[:, 0:64] = 256.0 * (np.arange(64) + 1)
    iot[:, 64:128] = 256.0 * np.arange(64)
    iot[:, 128:384] = 64.0 * np.arange(256)
    identb = np.eye(128, dtype=f32).astype(BF)
    k_ = np.arange(128)[:, None]
    q_ = np.arange(512)[None, :]
    for core in range(8):
        b, r = core // 4, core % 4
        tok = ((4 * np.arange(NSLOT)[:, None] + r) * 512 + np.arange(512)[None, :]).reshape(-1)
        m = {}
        m["xT"] = np.ascontiguousarray(np.asarray(inp["x"][b], f32)[tok].T)
        m["memT"] = np.ascontiguousarray(np.asarray(inp["mem"][b], f32).T)
        for nm, key in (("w_in", "w_in"), ("w_out", "w_out"), ("w_up", "ffn_w_up"), ("w_down", "ffn_w_down"), ("w_mkv", "w_mem_kv")):
            w = np.asarray(inp[key], f32)
            rows = w.shape[1] // 4
            CR = min(128, rows)
            w4 = w.reshape(w.shape[0], rows // CR, 4, CR, w.shape[2])
            m[nm] = np.ascontiguousarray(w4[:, :, r].reshape(w.shape[0], rows, w.shape[2]))
        m["lnp"], m["convp"], m["bfg"], m["peT"], m["cw1"], m["cw2"] = lnp, convp, bfg, peT, cw1, cw2
        thi, tlo = (tok // 128) * 128.0, (tok % 128) * 1.0
        qp = np.zeros((HM + HN, 4, NT), f32)
        for h in range(HM + HN):
            qp[h, 0], qp[h, 1], qp[h, 2], qp[h, 3] = -slopes[h] * thi, -slopes[h] * tlo, slopes[h], slopes[h]
        m["qpos"] = qp.astype(BF)
        m["kpos"] = np.stack([np.ones(NT), np.ones(NT), thi, tlo]).astype(BF)
        m["kcpos"] = kcpos
        m["fq3"] = np.full((3, NT), -1.0, f32).astype(BF)
        m["fk3"] = np.full((3, NT), 1.0, f32).astype(BF)
        cm = np.zeros((128, 16, 512), f32)
        for i in range(16):
            cm[:, i, :] = np.where(128 * i + k_ <= 512 * r + q_, 0.0, NEG)
        wm = np.zeros((128, 20, 512), f32)
        for i in range(20):
            dd = (512 * r + q_) - (128 * (i - 4) + k_)
            wm[:, i, :] = np.where((dd >= 0) & (dd < 512), 0.0, NEG)
        cp = np.zeros((128, 2, 512), f32)
        for i in range(2):
            cp[:, i, :] = np.where(16 * (128 * (i - 1) + k_) + 31 <= 512 * r + q_, 0.0, NEG)
        cpt = np.zeros((128, 4, 256), f32)
        qq = np.arange(128)[:, None]
        nn = np.arange(256)[None, :]
        for qt in range(4):
            cpt[:, qt, :] = np.where(16 * (nn - 128) + 31 <= 512 * r + 128 * qt + qq, 0.0, NEG)
        m["cmt"] = cm.reshape(128, -1).astype(BF)
        m["wmt"] = wm.reshape(128, -1).astype(BF)
        m["cpm"] = cp.reshape(128, -1).astype(BF)
        m["cpmt"] = cpt.reshape(128, -1).astype(BF)
        post = np.zeros((128, 2 * NSLOT * 4), f32)
        for j in range(NSLOT):
            for qt in range(4):
                t = (4 * j + r) * 512 + qt * 128 + np.arange(128)
                post[:, j * 4 + qt] = 256.0 * (t // 256)
                post[:, NSLOT * 4 + j * 4 + qt] = 64.0 * (t // 64)
        m["post"] = post
        m["iot"] = iot
        m["identb"] = identb
        oh = np.zeros((128, 8), f32)
        oh[:, r] = 1.0
        if r >= 1:
            oh[:, 4 + r - 1] = 1.0
        else:
            oh[:, 7] = 1.0
        m["ohsel"] = oh
        in_maps.append(m)
    return in_maps


_CACHE = {}


def run_cfg(c, inp):
    key = (c.D, c.S, c.HM, c.HN, c.HF, c.HE, c.DFF)
    if key not in _CACHE:
        _CACHE[key] = build(c)[0]
    nc = _CACHE[key]
    in_maps = host_inputs(c, inp)
    res = run_bass_kernel_spmd(nc, in_maps, core_ids=list(range(8)))
    out = np.zeros((2, c.S, c.D), np.float32)
    for core in range(8):
        b, r = core // 4, core % 4
        tok = ((4 * np.arange(c.NSLOT)[:, None] + r) * 512 + np.arange(512)[None, :]).reshape(-1)
        out[b, tok, :] = np.asarray(res.results[core]["out"]).T
    return out


def kernel(**inputs):
    return run_cfg(Cfg(), inputs)
```
